# Optimizing a Trainium2 kernel written in Bass

```python
import jax, jax.numpy as jnp
from jax import lax
import numpy as np

D_MODEL = 2048
BATCH = 4
SEQ = 4096
DEPTH = 2

CTX_LEN = 256
GRID_W = 64
N_MIXERS = 2
NORM_EPS = 1e-6
N_MOD = 6

MLSTM_HEADS = 8
MLSTM_QK_DIM = D_MODEL // 2 // MLSTM_HEADS
MLSTM_V_DIM = D_MODEL // MLSTM_HEADS
MLSTM_CHUNK = 64
GATE_SOFTCAP = 15.0
MLSTM_IN_COLS = 2 * MLSTM_HEADS * MLSTM_QK_DIM + 2 * MLSTM_HEADS * MLSTM_V_DIM + 4 * MLSTM_HEADS

ATTN_HEAD_DIM = 128
ATTN_Q_HEADS = D_MODEL // ATTN_HEAD_DIM
ATTN_KV_HEADS = 4
ATTN_QKV_COLS = (ATTN_Q_HEADS + 2 * ATTN_KV_HEADS) * ATTN_HEAD_DIM
Q_BLOCK = 128
ROPE_THETA = 10000.0

FFN_HIDDEN = -(-8 * D_MODEL // (3 * 256)) * 256

kernel_name = "hybrid_mlstm_gqa_prefix_dit"


def rms_norm(x, w):
    xf = x.astype(jnp.float32)
    y = xf * lax.rsqrt(jnp.mean(xf * xf, axis=-1, keepdims=True) + NORM_EPS)
    return (y * w.astype(jnp.float32)).astype(x.dtype)


def adaln(x, g, shift, scale):
    return rms_norm(x, g) * (1 + scale) + shift


def swiglu(h, w_in, w_out):
    gate, up = jnp.split(h @ w_in, 2, axis=-1)
    return (jax.nn.silu(gate) * up) @ w_out


def softcap(a, cap):
    return cap * jnp.tanh(a / cap)


def mlstm_project(h, w_in, b_gate):
    H, dk, dv = MLSTM_HEADS, MLSTM_QK_DIM, MLSTM_V_DIM
    p = h @ w_in
    B, T, _ = p.shape
    q, k, v, o, g = jnp.split(p, [H * dk, 2 * H * dk, 2 * H * dk + H * dv, 2 * H * dk + 2 * H * dv], axis=-1)
    q = q.reshape(B, T, H, dk).transpose(0, 2, 1, 3) * (dk ** -0.5)
    k = k.reshape(B, T, H, dk).transpose(0, 2, 1, 3)
    v = v.reshape(B, T, H, dv).transpose(0, 2, 1, 3)
    g = softcap(g.astype(jnp.float32) + b_gate.astype(jnp.float32), GATE_SOFTCAP)
    g = g.reshape(B, T, 4, H).transpose(2, 0, 3, 1)
    return q, k, v, o, g[0], jax.nn.log_sigmoid(g[1]), g[2], jax.nn.log_sigmoid(g[3])


def mlstm_scan(q, k, v, ig, lf, state):
    B, H, T, dk = q.shape
    dv = v.shape[-1]
    L = MLSTM_CHUNK
    nc = T // L

    def chunks(a):
        a = a.astype(jnp.float32).reshape(B, H, nc, L, *a.shape[3:])
        return jnp.moveaxis(a, 2, 0)

    lower = jnp.tril(jnp.ones((L, L), dtype=bool))

    def body(carry, inp):
        C, n, m = carry
        qc, kc, vc, ic, fc = inp
        b = jnp.cumsum(fc, axis=-1)
        log_d = b[..., :, None] - b[..., None, :] + ic[..., None, :]
        log_d = jnp.where(lower, log_d, -jnp.inf)
        log_inter = b + m[..., None]
        m_t = jnp.maximum(log_inter, jnp.max(log_d, axis=-1))
        dmat = jnp.exp(log_d - m_t[..., None])
        inter = jnp.exp(log_inter - m_t)
        sm = jnp.einsum('bhtd,bhsd->bhts', qc, kc) * dmat
        num = jnp.einsum('bhts,bhsv->bhtv', sm, vc) + inter[..., None] * jnp.einsum('bhtd,bhdv->bhtv', qc, C)
        den = jnp.sum(sm, axis=-1) + inter * jnp.einsum('bhtd,bhd->bht', qc, n)
        h = num / jnp.maximum(jnp.abs(den), jnp.exp(-m_t))[..., None]
        b_end = b[..., -1]
        log_w = b_end[..., None] - b + ic
        m_new = jnp.maximum(b_end + m, jnp.max(log_w, axis=-1))
        w = jnp.exp(log_w - m_new[..., None])
        decay = jnp.exp(b_end + m - m_new)
        C_new = decay[..., None, None] * C + jnp.einsum('bhs,bhsd,bhsv->bhdv', w, kc, vc)
        n_new = decay[..., None] * n + jnp.einsum('bhs,bhsd->bhd', w, kc)
        return (C_new, n_new, m_new), h

    state, hs = lax.scan(body, state, (chunks(q), chunks(k), chunks(v), chunks(ig), chunks(lf)))
    h = jnp.moveaxis(hs, 0, 2).reshape(B, H, T, dv)
    return state, h


def mlstm_out(h, o, g_head, w_out):
    B, H, T, dv = h.shape
    hn = h * lax.rsqrt(jnp.mean(h * h, axis=-1, keepdims=True) + NORM_EPS)
    hn = hn * g_head.astype(jnp.float32).reshape(1, H, 1, dv)
    hn = hn.transpose(0, 2, 1, 3).reshape(B, T, H * dv).astype(o.dtype)
    return (jax.nn.sigmoid(o) * hn) @ w_out


def mlstm_mixer(h_lat, h_ctx, w_in, b_gate, g_head, w_out, with_ctx_out):
    ql, kl, vl, ol, il_f, fl_f, il_b, fl_b = mlstm_project(h_lat, w_in, b_gate)
    qc, kc, vc, oc, ic_f, fc_f, ic_b, fc_b = mlstm_project(h_ctx, w_in, b_gate)
    B = h_lat.shape[0]
    H, dk, dv = MLSTM_HEADS, MLSTM_QK_DIM, MLSTM_V_DIM
    zero = (jnp.zeros((B, H, dk, dv), jnp.float32), jnp.zeros((B, H, dk), jnp.float32),
            jnp.zeros((B, H), jnp.float32))
    rev = lambda a: jnp.flip(a, axis=2)
    st_f, hc_f = mlstm_scan(qc, kc, vc, ic_f, fc_f, zero)
    st_b, hc_b = mlstm_scan(rev(qc), rev(kc), rev(vc), rev(ic_b), rev(fc_b), zero)
    _, hl_f = mlstm_scan(ql, kl, vl, il_f, fl_f, st_f)
    _, hl_b = mlstm_scan(rev(ql), rev(kl), rev(vl), rev(il_b), rev(fl_b), st_b)
    y_lat = mlstm_out(hl_f + rev(hl_b), ol, g_head, w_out)
    y_ctx = mlstm_out(hc_f + rev(hc_b), oc, g_head, w_out) if with_ctx_out else None
    return y_lat, y_ctx


def axial_rope_tables(n_tokens):
    rows = n_tokens // GRID_W
    t_row = jnp.repeat(jnp.arange(rows, dtype=jnp.float32), GRID_W)
    t_col = jnp.tile(jnp.arange(GRID_W, dtype=jnp.float32), rows)
    per_axis = ATTN_HEAD_DIM // 2
    inv = ROPE_THETA ** (-jnp.arange(0, per_axis, 2, dtype=jnp.float32) / per_axis)
    ang = jnp.concatenate([t_row[:, None] * inv, t_col[:, None] * inv], axis=-1)
    return jnp.cos(ang), jnp.sin(ang)


def apply_rope(x, cos, sin):
    xp = x.astype(jnp.float32).reshape(*x.shape[:-1], x.shape[-1] // 2, 2)
    x0, x1 = xp[..., 0], xp[..., 1]
    c = cos[None, :, None, :]
    s = sin[None, :, None, :]
    out = jnp.stack([x0 * c - x1 * s, x0 * s + x1 * c], axis=-1)
    return out.reshape(x.shape).astype(x.dtype)


def gqa_attend(q, k, v):
    s = jnp.einsum('bqkgd,bskd->bkgqs', q, k) * (ATTN_HEAD_DIM ** -0.5)
    p = jax.nn.softmax(s.astype(jnp.float32), axis=-1).astype(v.dtype)
    return jnp.einsum('bkgqs,bskd->bqkgd', p, v)


def attn_mixer(h_lat, h_ctx, w_qkv, g_q, g_k, w_out, with_ctx_out):
    Hq, Hkv, dh = ATTN_Q_HEADS, ATTN_KV_HEADS, ATTN_HEAD_DIM
    G = Hq // Hkv

    def qkv(h):
        p = h @ w_qkv
        B, T, _ = p.shape
        q, k, v = jnp.split(p, [Hq * dh, (Hq + Hkv) * dh], axis=-1)
        q = rms_norm(q.reshape(B, T, Hq, dh), g_q)
        k = rms_norm(k.reshape(B, T, Hkv, dh), g_k)
        return q, k, v.reshape(B, T, Hkv, dh)

    ql, kl, vl = qkv(h_lat)
    qc, kc, vc = qkv(h_ctx)
    B, T = h_lat.shape[:2]
    Lc = h_ctx.shape[1]
    cos, sin = axial_rope_tables(T)
    ql = apply_rope(ql, cos, sin)
    kl = apply_rope(kl, cos, sin)
    k_all = jnp.concatenate([kl, kc], axis=1)
    v_all = jnp.concatenate([vl, vc], axis=1)
    nb = T // Q_BLOCK
    qb = ql.reshape(B, nb, Q_BLOCK, Hkv, G, dh).transpose(1, 0, 2, 3, 4, 5)
    o_lat = lax.map(lambda qblk: gqa_attend(qblk, k_all, v_all), qb)
    o_lat = o_lat.transpose(1, 0, 2, 3, 4, 5).reshape(B, T, Hq * dh)
    y_lat = o_lat @ w_out
    if with_ctx_out:
        o_ctx = gqa_attend(qc.reshape(B, Lc, Hkv, G, dh), kc, vc).reshape(B, Lc, Hq * dh)
        y_ctx = o_ctx @ w_out
    else:
        y_ctx = None
    return y_lat, y_ctx


def setup_inputs(seed: int = 0) -> dict:
    key = jax.random.key(seed)
    ks = jax.random.split(key, 24)
    f32 = jnp.float32
    D, F = D_MODEL, FFN_HIDDEN
    n_a = (DEPTH + N_MIXERS - 1) // N_MIXERS
    n_b = DEPTH // N_MIXERS
    H = MLSTM_HEADS
    nrm = lambda k, shape, scale: jax.random.normal(k, shape, f32) * scale
    gain = lambda k, shape: 1.0 + 0.05 * jax.random.normal(k, shape, f32)
    fg_bias = jnp.linspace(3.0, 6.0, H, dtype=f32)
    gate_off = jnp.stack([jnp.zeros((H,), f32), fg_bias, jnp.zeros((H,), f32), fg_bias])
    b_gate = (0.1 * jax.random.normal(ks[10], (n_a, 4, H), f32) + gate_off).reshape(n_a, 4 * H)
    return {
        "x": jax.random.normal(ks[0], (BATCH, SEQ, D), f32),
        "c": jax.random.normal(ks[1], (BATCH, D), f32),
        "ctx": jax.random.normal(ks[2], (BATCH, CTX_LEN, D), f32),
        "c_ctx": jax.random.normal(ks[3], (D,), f32),
        "w_mod": nrm(ks[4], (DEPTH, D, N_MOD * D), 0.5 * D ** -0.5),
        "b_mod": nrm(ks[5], (DEPTH, N_MOD * D), 0.02),
        "g_mix_pre": gain(ks[6], (DEPTH, D)),
        "g_mix_post": gain(ks[7], (DEPTH, D)),
        "g_ffn_pre": gain(ks[8], (DEPTH, D)),
        "g_ffn_post": gain(ks[9], (DEPTH, D)),
        "w_mlstm_in": nrm(ks[11], (n_a, D, MLSTM_IN_COLS), D ** -0.5),
        "b_mlstm_gate": b_gate,
        "g_mlstm_head": gain(ks[12], (n_a, H * MLSTM_V_DIM)),
        "w_mlstm_out": nrm(ks[13], (n_a, H * MLSTM_V_DIM, D), (H * MLSTM_V_DIM) ** -0.5),
        "w_attn_qkv": nrm(ks[14], (n_b, D, ATTN_QKV_COLS), D ** -0.5),
        "g_attn_q": gain(ks[15], (n_b, ATTN_HEAD_DIM)),
        "g_attn_k": gain(ks[16], (n_b, ATTN_HEAD_DIM)),
        "w_attn_out": nrm(ks[17], (n_b, ATTN_Q_HEADS * ATTN_HEAD_DIM, D), (ATTN_Q_HEADS * ATTN_HEAD_DIM) ** -0.5),
        "w_ffn_in": nrm(ks[18], (DEPTH, D, 2 * F), D ** -0.5),
        "w_ffn_out": nrm(ks[19], (DEPTH, F, D), F ** -0.5),
    }


def reference(x, c, ctx, c_ctx, w_mod, b_mod, g_mix_pre, g_mix_post, g_ffn_pre, g_ffn_post,
              w_mlstm_in, b_mlstm_gate, g_mlstm_head, w_mlstm_out,
              w_attn_qkv, g_attn_q, g_attn_k, w_attn_out, w_ffn_in, w_ffn_out):
    cond_lat = jax.nn.silu(c)
    cond_ctx = jax.nn.silu(c_ctx)
    for i in range(DEPTH):
        last = i == DEPTH - 1
        j = i // N_MIXERS
        sh1, sc1, gt1, sh2, sc2, gt2 = jnp.split((cond_lat @ w_mod[i] + b_mod[i])[:, None, :], N_MOD, axis=-1)
        csh1, csc1, cgt1, csh2, csc2, cgt2 = jnp.split(cond_ctx @ w_mod[i] + b_mod[i], N_MOD, axis=-1)
        h_lat = adaln(x, g_mix_pre[i], sh1, sc1)
        h_ctx = adaln(ctx, g_mix_pre[i], csh1, csc1)
        if i % N_MIXERS == 0:
            y_lat, y_ctx = mlstm_mixer(h_lat, h_ctx, w_mlstm_in[j], b_mlstm_gate[j], g_mlstm_head[j],
                                       w_mlstm_out[j], not last)
        else:
            y_lat, y_ctx = attn_mixer(h_lat, h_ctx, w_attn_qkv[j], g_attn_q[j], g_attn_k[j],
                                      w_attn_out[j], not last)
        x = x + gt1 * rms_norm(y_lat, g_mix_post[i])
        f_lat = swiglu(adaln(x, g_ffn_pre[i], sh2, sc2), w_ffn_in[i], w_ffn_out[i])
        x = x + gt2 * rms_norm(f_lat, g_ffn_post[i])
        if not last:
            ctx = ctx + cgt1 * rms_norm(y_ctx, g_mix_post[i])
            f_ctx = swiglu(adaln(ctx, g_ffn_pre[i], csh2, csc2), w_ffn_in[i], w_ffn_out[i])
            ctx = ctx + cgt2 * rms_norm(f_ctx, g_ffn_post[i])
    return x
```

```python
import numpy as np
import concourse.bass as bass
import concourse.mybir as mybir
from concourse.bass_utils import run_bass_kernel_spmd

F32 = mybir.dt.float32
BF16 = mybir.dt.bfloat16
AF = mybir.ActivationFunctionType
ALU = mybir.AluOpType
AX = mybir.AxisListType

D = 2048
KC = 16
NLAT = 2048
NCTX = 256
NTOK = NLAT + NCTX
NCHUNK = NTOK // 64
FH = 5632
FC = 44
EPS = 1e-6
NEG = -1.0e30
STREAMS = ("pe", "act", "dve", "pool", "sp")
DMA_RING = 8


class _Op:
    __slots__ = ("stream", "fn", "dma", "deps", "signal", "ev", "waits", "idx", "dma_i", "inc")

    def __init__(self, stream, fn, dma, idx):
        self.stream = stream
        self.fn = fn
        self.dma = dma
        self.deps = set()
        self.signal = dma
        self.ev = None
        self.waits = None
        self.idx = idx
        self.dma_i = None
        self.inc = 16 if dma else 1


class Sched:
    def __init__(self, nc):
        self.nc = nc
        self.ops = []
        self.by_stream = {s: [] for s in STREAMS}
        self.last_w = {}
        self.readers = {}
        self.floor = -1
        self.n_dma = {s: 0 for s in STREAMS}
        self.recent_dma = {s: [] for s in STREAMS}

    def add(self, stream, fn, reads=(), writes=(), dma=False):
        idx = len(self.ops)
        op = _Op(stream, fn, dma, idx)
        raw = set()
        other = set()
        ps_reads = [k for k in reads if isinstance(k, tuple) and k[0] == "ps"]
        if ps_reads:
            for k in ps_reads:
                w = self.last_w.get(k)
                if w is not None:
                    raw.add(w)
            reads = [k for k in reads if not (isinstance(k, tuple) and k[0] == "ps")]
            writes = list(writes) + ps_reads
        for k in reads:
            w = self.last_w.get(k)
            if w is not None:
                raw.add(w)
        for k in writes:
            w = self.last_w.get(k)
            if w is not None:
                other.add(w)
            rs = self.readers.get(k)
            if rs:
                other.update(rs)
        best = {}
        for j in raw | other:
            if j <= self.floor:
                continue
            o = self.ops[j]
            if o.dma:
                op.deps.add(j)
                continue
            if o.stream == stream and not dma:
                if stream == "pe":
                    continue
                if j not in raw:
                    continue
            if best.get(o.stream, -1) < j:
                best[o.stream] = j
        op.deps.update(best.values())
        for k in reads:
            rl = self.readers.setdefault(k, [])
            if not dma:
                rl[:] = [r for r in rl if self.ops[r].dma or self.ops[r].stream != stream]
            rl.append(idx)
        for k in writes:
            self.last_w[k] = idx
            self.readers[k] = []
        self.ops.append(op)
        self.by_stream[stream].append(op)
        if dma:
            op.dma_i = self.n_dma[stream]
            self.n_dma[stream] += 1
            rd = self.recent_dma[stream]
            rd.append(idx)
            if len(rd) > DMA_RING:
                rd.pop(0)
        return op

    def barrier(self):
        lasts = set()
        for s in STREAMS:
            for o in reversed(self.by_stream[s]):
                if not o.dma and o.fn is not None:
                    lasts.add(o.idx)
                    break
            lasts.update(self.recent_dma[s])
        first = len(self.ops)
        for s in STREAMS:
            op = _Op(s, None, False, len(self.ops))
            op.deps = set(j for j in lasts if j > self.floor)
            self.ops.append(op)
            self.by_stream[s].append(op)
        self.floor = first - 1
        self.last_w.clear()
        self.readers.clear()

    def emit(self):
        nc = self.nc
        ops = self.ops
        for op in ops:
            for j in op.deps:
                ops[j].signal = True
        comp_sem = {}
        ring = {}
        for s in STREAMS:
            if any((not o.dma) and o.signal for o in self.by_stream[s]):
                comp_sem[s] = nc.alloc_semaphore(name=f"c_{s}")
            if self.n_dma[s]:
                ring[s] = [nc.alloc_semaphore(name=f"d_{s}{i}") for i in range(DMA_RING)]
        for s in STREAMS:
            cnt = 0
            for o in self.by_stream[s]:
                if o.dma:
                    o.ev = (ring[s][o.dma_i % DMA_RING], 16 * (o.dma_i // DMA_RING + 1))
                elif o.signal:
                    cnt += 1
                    o.ev = (comp_sem[s], cnt)
        for s in STREAMS:
            waited = {}
            for o in self.by_stream[s]:
                need = {}
                for j in o.deps:
                    sem, val = ops[j].ev
                    key = id(sem)
                    if need.get(key, (None, 0))[1] < val:
                        need[key] = (sem, val)
                if o.dma and o.dma_i >= DMA_RING:
                    sem, val = o.ev[0], o.ev[1] - 16
                    key = id(sem)
                    if need.get(key, (None, 0))[1] < val:
                        need[key] = (sem, val)
                w = []
                for key, (sem, val) in need.items():
                    if waited.get(key, 0) >= val:
                        continue
                    waited[key] = val
                    w.append((sem, val))
                o.waits = w

        def run(stream):
            def body(eng):
                for o in self.by_stream[stream]:
                    for sem, val in o.waits:
                        eng.wait_ge(sem, val)
                    if o.fn is None:
                        continue
                    ins = o.fn(eng)
                    if o.signal:
                        ins.then_inc(o.ev[0], o.inc)
            return body

        with nc.Block() as block:
            block.tensor(run("pe"))
            block.scalar(run("act"))
            block.vector(run("dve"))
            block.gpsimd(run("pool"))
            block.sync(run("sp"))


class Arena:
    def __init__(self, nc, base=16640, limit=228352):
        self.nc = nc
        self.off = base
        self.limit = limit
        self.n = 0

    def alloc(self, shape, dtype, name="t"):
        nbytes = int(np.prod(shape[1:])) * mybir.dt.size(dtype)
        nbytes = (nbytes + 63) // 64 * 64
        off = self.off
        assert off + nbytes <= self.limit, f"SBUF arena overflow {name} {off}+{nbytes}"
        self.off += nbytes
        self.n += 1
        return self.nc.alloc_sbuf_tensor_at(f"{name}_{self.n}", list(shape), dtype, offset=off)

    def mark(self):
        return self.off

    def release(self, mark):
        self.off = mark


def bcast(ap, axis, shape):
    return ap.unsqueeze(axis).to_broadcast(list(shape))


class Prog:
    def __init__(self, stage):
        self.stage = stage
        self.nc = nc = bass.Bass("TRN2", target_bir_lowering=False)
        self.S = Sched(nc)
        self.A = Arena(nc)
        self.banks = [nc.alloc_psum_tensor(f"psb{i}", [128, 512], F32) for i in range(8)]
        self.rot = list(range(7))
        self.rot_i = 0
        self.wi = 0
        self.uid = 0
        self.io_in = []
        self.io_out = []

    def din(self, name, shape, dt=F32):
        self.io_in.append(name)
        return self.nc.dram_tensor(name, list(shape), dt, kind="ExternalInput").ap()

    def dout(self, name, shape, dt=F32):
        self.io_out.append(name)
        return self.nc.dram_tensor(name, list(shape), dt, kind="ExternalOutput").ap()

    def scr(self, name, shape, dt, prod, cons):
        st = self.stage
        if st == 0:
            return self.nc.dram_tensor(name, list(shape), dt, kind="Internal").ap()
        if st == prod:
            if any(c != prod for c in cons):
                return self.dout(name, shape, dt)
            return self.nc.dram_tensor(name, list(shape), dt, kind="Internal").ap()
        if st in cons:
            return self.din(name, shape, dt)
        return None

    def ps(self):
        b = self.rot[self.rot_i % len(self.rot)]
        self.rot_i += 1
        return self.banks[b], ("ps", b)

    def key(self, name):
        self.uid += 1
        return (name, self.uid)

    def mm(self, out, lhsT, rhs, start, stop, r, w):
        self.S.add("pe", lambda e: e.matmul(out, lhsT, rhs, start=start, stop=stop), reads=r, writes=w)

    def tr(self, out, in_, ident, r, w):
        self.S.add("pe", lambda e: e.transpose(out, in_, ident), reads=r, writes=w)

    def act(self, out, in_, func, r, w, bias=None, scale=None):
        kw = {}
        if bias is not None:
            kw["bias"] = bias
        if scale is not None:
            kw["scale"] = scale
        self.S.add("act", lambda e: e.activation(out=out, in_=in_, func=func, **kw), reads=r, writes=w)

    def tt(self, out, in0, in1, op, r, w, eng="dve"):
        self.S.add(eng, lambda e: e.tensor_tensor(out=out, in0=in0, in1=in1, op=op), reads=r, writes=w)

    def ts(self, out, in0, s1, s2, op0, op1, r, w, eng="dve"):
        if s2 is None:
            self.S.add(eng, lambda e: e.tensor_scalar(out=out, in0=in0, scalar1=s1, scalar2=None, op0=op0), reads=r, writes=w)
        else:
            self.S.add(eng, lambda e: e.tensor_scalar(out=out, in0=in0, scalar1=s1, scalar2=s2, op0=op0, op1=op1), reads=r, writes=w)

    def stt(self, out, in0, scalar, in1, op0, op1, r, w, eng="dve"):
        self.S.add(eng, lambda e: e.scalar_tensor_tensor(out=out, in0=in0, scalar=scalar, in1=in1, op0=op0, op1=op1), reads=r, writes=w)

    def rsq(self, ap, k):
        self.act(ap, ap, AF.Sqrt, k, k)
        self.S.add("dve", lambda e: e.reciprocal(out=ap, in_=ap), reads=k, writes=k)

    def cp(self, out, in_, r, w, eng="dve"):
        self.S.add(eng, lambda e: e.tensor_copy(out=out, in_=in_), reads=r, writes=w)

    def red(self, out, in_, op, r, w):
        self.S.add("dve", lambda e: e.tensor_reduce(out=out, in_=in_, axis=AX.X, op=op), reads=r, writes=w)

    def dma(self, out, in_, r, w, q="sp"):
        self.S.add(q, lambda e: e.dma_start(out=out, in_=in_), reads=r, writes=w, dma=True)

    def wload(self, W2d, kc, col0, ncols):
        i = self.wi % len(self.wbufs)
        self.wi += 1
        buf = self.wbufs[i]
        key = ("w", i)
        view = buf[:, 0:kc * ncols].rearrange("p (c n) -> p c n", c=kc)
        src = W2d.rearrange("(c p) n -> p c n", p=128)[:, :, col0:col0 + ncols]
        step = 16 if kc <= 16 else 11
        for k0 in range(0, kc, step):
            self.dma(view[:, k0:k0 + step, :], src[:, k0:k0 + step, :], [], [key], q="pool")
        return view, key

    def setup_consts(self):
        A = self.A
        cst = self.din("cst", [128, 128 * 7])
        self.cst_f = A.alloc([128, 128 * 7], F32, "cstf")
        self.dma(self.cst_f[:], cst, [], ["cstf"])
        self.cst_b = A.alloc([128, 256], BF16, "cstb")
        self.cp(self.cst_b[:, 0:128], self.cst_f[:, 0:128], ["cstf"], ["cstb"])
        self.cp(self.cst_b[:, 128:256], self.cst_f[:, 128:256], ["cstf"], ["cstb"])
        self.identb = A.alloc([128, 128], BF16, "identb")
        self.cp(self.identb[:], self.cst_f[:, 768:896], ["cstf"], ["cstb"])
        self.ones_f = self.cst_f[:, 0:128]
        self.ones_b = self.cst_b[:, 0:128]
        self.pswap_b = self.cst_b[:, 128:256]
        self.tri = [self.cst_f[0:64, 256:320], self.cst_f[0:64, 320:384]]
        self.nm = [self.cst_f[0:64, 384:448], self.cst_f[0:64, 448:512]]
        self.id64 = self.cst_f[0:64, 512:576]
        self.wbufs = [A.alloc([128, 8192], BF16, f"wbuf{i}") for i in range(3)]
        self.MOD = A.alloc([128, 2 * 2 * 6 * 16], F32, "MOD")

    def modv(self, l, v, kind):
        o = ((l * 2 + v) * 6 + kind) * 16
        return self.MOD[:, o:o + 16]

    def phase_mod(self, w_mod, b_mod, gvec, cond, s_mod):
        A = self.A
        mk = A.mark()
        cf = A.alloc([128, 32], F32, "cf")
        cb = A.alloc([128, 32], BF16, "cb")
        bm = A.alloc([128, 192], F32, "bm")
        gv = A.alloc([128, 128], F32, "gv")
        raw = A.alloc([128, 2 * 192], F32, "raw")
        self.dma(cf[:], cond, [], ["cf"])
        self.dma(bm[:], b_mod, [], ["bm"])
        self.dma(gv[:], gvec, [], ["gv"])
        self.act(cb[:], cf[:], AF.Silu, ["cf"], ["cb"])
        cbv = cb[:].rearrange("p (c j) -> p c j", j=2)
        for l in range(2):
            bank, bk = self.banks[7], ("ps", 7)
            for t in range(24):
                wv, wk = self.wload(w_mod[l], KC, t * 512, 512)
                for jj in range(4):
                    j = t * 4 + jj
                    for kc in range(KC):
                        self.mm(bank[:, 2 * j:2 * j + 2], wv[:, kc, jj * 128:(jj + 1) * 128], cbv[:, kc, :],
                                kc == 0, kc == KC - 1, [wk, "cb"], [bk])
            rw = raw[:, l * 192:(l + 1) * 192].rearrange("p (j v) -> p j v", v=2)
            self.tt(rw, bank[:, 0:192].rearrange("p (j v) -> p j v", v=2),
                    bcast(bm[:, l * 96:(l + 1) * 96], 2, [128, 96, 2]), ALU.add, [bk, "bm"], ["raw"])
            for v in range(2):
                def part(q):
                    return raw[:, l * 192:(l + 1) * 192].rearrange("p (q c v) -> p q c v", q=6, v=2)[:, q, :, v]
                g = lambda i: gv[:, (i * 2 + l) * 16:(i * 2 + l) * 16 + 16]
                self.stt(self.modv(l, v, 0), part(1), 1.0, g(0), ALU.add, ALU.mult, ["raw", "gv"], ["MOD"])
                self.cp(self.modv(l, v, 1), part(0), ["raw"], ["MOD"])
                self.tt(self.modv(l, v, 2), part(2), g(1), ALU.mult, ["raw", "gv"], ["MOD"])
                self.stt(self.modv(l, v, 3), part(4), 1.0, g(2), ALU.add, ALU.mult, ["raw", "gv"], ["MOD"])
                self.cp(self.modv(l, v, 4), part(3), ["raw"], ["MOD"])
                self.tt(self.modv(l, v, 5), part(5), g(3), ALU.mult, ["raw", "gv"], ["MOD"])
        if s_mod is not None:
            self.dma(s_mod, self.MOD[:], ["MOD"], ["s_mod"])
        self.S.barrier()
        A.release(mk)

    def adaln(self, X, xk, Y, yk, HB, hk, T, a_ap, sh_ap, modk="MOD"):
        sb, sk = self.banks[7], ("ps", 7)
        for c in range(KC):
            sq = self.sqc[c % 2]
            sqk = ("sqc", c % 2)
            self.act(sq[:, 0:T], X[:, c, 0:T], AF.Square, [xk], [sqk])
            self.mm(sb[:, 0:T], self.ones_b, sq[:, 0:T], c == 0, c == KC - 1, [sqk], [sk])
        rs = self.rstd
        self.ts(rs[:, 0:T], sb[:, 0:T], 1.0 / D, EPS, ALU.mult, ALU.add, [sk], ["rstd"])
        self.rsq(rs[:, 0:T], ["rstd"])
        yks = [(yk, c) for c in range(KC)]
        self.tt(Y[:, :, 0:T], X[:, :, 0:T], bcast(rs[:, 0:T], 1, [128, KC, T]), ALU.mult, [xk, "rstd"], yks)
        for c in range(KC):
            self.act(HB[:, c, 0:T], Y[:, c, 0:T], AF.Identity, [(yk, c), modk], [(hk, c)],
                     bias=sh_ap[:, c:c + 1], scale=a_ap[:, c:c + 1])

    def linear_post_res(self, HBin, hk, kcn, W2d, X, xk, Y, yk, T, gp_ap):
        sb, sk = self.banks[7], ("ps", 7)
        ncols = 512 if kcn == KC else 128
        per = ncols // 128
        for t in range(D // ncols):
            wv, wk = self.wload(W2d, kcn, t * ncols, ncols)
            for jj in range(per):
                c = t * per + jj
                bank, bk = self.ps()
                for kc in range(kcn):
                    self.mm(bank[:, 0:T], wv[:, kc, jj * 128:(jj + 1) * 128], HBin[:, kc, 0:T],
                            kc == 0, kc == kcn - 1, [wk, (hk, kc)], [bk])
                self.cp(Y[:, c, 0:T], bank[:, 0:T], [bk], [(yk, c)])
                sq = self.sqc[c % 2]
                sqk = ("sqc", c % 2)
                self.act(sq[:, 0:T], bank[:, 0:T], AF.Square, [bk], [sqk])
                self.mm(sb[:, 0:T], self.ones_b, sq[:, 0:T], c == 0, c == KC - 1, [sqk], [sk])
        rs = self.rstd
        self.ts(rs[:, 0:T], sb[:, 0:T], 1.0 / D, EPS, ALU.mult, ALU.add, [sk], ["rstd"])
        self.rsq(rs[:, 0:T], ["rstd"])
        yks = [(yk, c) for c in range(KC)]
        self.tt(Y[:, :, 0:T], Y[:, :, 0:T], bcast(rs[:, 0:T], 1, [128, KC, T]), ALU.mult, yks + ["rstd"], yks)
        for c in range(KC):
            self.stt(X[:, c, 0:T], Y[:, c, 0:T], gp_ap[:, c:c + 1], X[:, c, 0:T], ALU.mult, ALU.add,
                     [(yk, c), xk, "MOD"], [xk])

    def ffn(self, HB, hk, ACT_T, ak, w_in, w_out, X, xk, Y, yk, T, gp_ap):
        for g in range(11):
            wg, wgk = self.wload(w_in, KC, g * 512, 512)
            wu, wuk = self.wload(w_in, KC, FH + g * 512, 512)
            for jj in range(4):
                f = g * 4 + jj
                bg, bgk = self.ps()
                for kc in range(KC):
                    self.mm(bg[:, 0:T], wg[:, kc, jj * 128:(jj + 1) * 128], HB[:, kc, 0:T], kc == 0, kc == KC - 1, [wgk, (hk, kc)], [bgk])
                bu, buk = self.ps()
                for kc in range(KC):
                    self.mm(bu[:, 0:T], wu[:, kc, jj * 128:(jj + 1) * 128], HB[:, kc, 0:T], kc == 0, kc == KC - 1, [wuk, (hk, kc)], [buk])
                sg = self.sgb[f % 2]
                sgk = ("sgb", f % 2)
                self.act(sg[:, 0:T], bg[:, 0:T], AF.Silu, [bgk], [sgk])
                self.tt(ACT_T[:, f, 0:T], bu[:, 0:T], sg[:, 0:T], ALU.mult, [buk, sgk], [(ak, f)])
        self.linear_post_res(ACT_T, ak, FC, w_out, X, xk, Y, yk, T, gp_ap)


def tile_info(ti):
    if ti == 0:
        return NCTX, 0
    return 512, NCTX + (ti - 1) * 512


def load_x_tile(P, X, xk, xT, ctxT, ti):
    T, g0 = tile_info(ti)
    if ti == 0:
        src = ctxT.rearrange("(c p) t -> p c t", p=128)
    else:
        src = xT.rearrange("(c p) t -> p c t", p=128)[:, :, (ti - 1) * 512:ti * 512]
    P.dma(X[:, :, 0:T], src, [], [xk])


def alloc_common(P):
    A = P.A
    P.sqc = [A.alloc([128, 512], BF16, f"sqc{i}") for i in range(2)]
    P.rstd = A.alloc([128, 512], F32, "rstd")


def pass1(P, xT, ctxT, w_in, bgate, sc):
    A = P.A
    mk = A.mark()
    X = A.alloc([128, KC, 512], F32, "X")
    Y = A.alloc([128, KC, 512], F32, "Y")
    HB = A.alloc([128, KC, 512], BF16, "HB")
    qT = A.alloc([128, 8, 8, 64], BF16, "qT")
    kT = A.alloc([128, 8, 8, 64], BF16, "kT")
    st = [A.alloc([128, 512], BF16, f"st{i}") for i in range(4)]
    bg = A.alloc([128, 32], F32, "bg")
    gt = [A.alloc([128, 32], F32, f"gt{i}") for i in range(4)]
    P.dma(bg[:], bgate.partition_broadcast(128).rearrange("p a n -> p (a n)"), [], ["bg"])
    sti = 0
    for ti in range(5):
        T, g0 = tile_info(ti)
        nch, nblk = T // 64, T // 128
        v = 1 if ti == 0 else 0
        load_x_tile(P, X, "X", xT, ctxT, ti)
        P.adaln(X, "X", Y, "Y", HB, "HB", T, P.modv(0, v, 0), P.modv(0, v, 1))
        for t in range(4):
            wv, wk = P.wload(w_in, KC, t * 512, 512)
            for jj in range(4):
                hh = (t % 2) * 4 + jj
                bank, bk = P.ps()
                for kc in range(KC):
                    P.mm(bank[:, 0:T], wv[:, kc, jj * 128:(jj + 1) * 128], HB[:, kc, 0:T], kc == 0, kc == KC - 1,
                         [wk, ("HB", kc)], [bk])
                dst = (qT if t < 2 else kT)
                dk = "qT" if t < 2 else "kT"
                src = bank[:, 0:T].rearrange("p (c t) -> p c t", t=64)
                if t < 2:
                    P.ts(dst[:, 0:nch, hh, :], src, 128.0 ** -0.5, None, ALU.mult, None, [bk], [dk])
                else:
                    P.cp(dst[:, 0:nch, hh, :], src, [bk], [dk])
            if t >= 2:
                for blk in range(nblk):
                    bank, bk = P.ps()
                    for kc in range(KC):
                        P.mm(bank[:, :], HB[:, kc, blk * 128:(blk + 1) * 128], wv[:, kc, :], kc == 0, kc == KC - 1,
                             [wk, ("HB", kc)], [bk])
                    s_ = st[sti % 4]
                    sk_ = ("st", sti % 4)
                    sti += 1
                    P.act(s_[:], bank[:, :], AF.Copy, [bk], [sk_])
                    r0 = g0 + blk * 128
                    P.dma(sc["ktm"][r0:r0 + 128, (t - 2) * 512:(t - 1) * 512], s_[:], [sk_], [])
        c0 = g0 // 64
        P.dma(sc["qT"][c0:c0 + nch].rearrange("c p h t -> p c h t"), qT[:, 0:nch], ["qT"], [])
        P.dma(sc["kT"][c0:c0 + nch].rearrange("c p h t -> p c h t"), kT[:, 0:nch], ["kT"], [])
        for t in range(4, 12):
            wv, wk = P.wload(w_in, KC, t * 512, 512)
            for blk in range(nblk):
                bank, bk = P.ps()
                for kc in range(KC):
                    P.mm(bank[:, :], HB[:, kc, blk * 128:(blk + 1) * 128], wv[:, kc, :], kc == 0, kc == KC - 1,
                         [wk, ("HB", kc)], [bk])
                s_ = st[sti % 4]
                sk_ = ("st", sti % 4)
                sti += 1
                r0 = g0 + blk * 128
                if t < 8:
                    P.cp(s_[:], bank[:, :], [bk], [sk_])
                    P.dma(sc["vtm"][r0:r0 + 128, (t - 4) * 512:(t - 3) * 512], s_[:], [sk_], [])
                else:
                    P.act(s_[:], bank[:, :], AF.Sigmoid, [bk], [sk_])
                    P.dma(sc["sgo"][r0:r0 + 128, (t - 8) * 512:(t - 7) * 512], s_[:], [sk_], [])
        wv, wk = P.wload(w_in, KC, 6144, 32)
        for blk in range(nblk):
            bank, bk = P.ps()
            for kc in range(KC):
                P.mm(bank[:, 0:32], HB[:, kc, blk * 128:(blk + 1) * 128], wv[:, kc, :], kc == 0, kc == KC - 1,
                     [wk, ("HB", kc)], [bk])
            g1, g2, g3, go = gt
            P.tt(g1[:], bank[:, 0:32], bg[:], ALU.add, [bk, "bg"], ["g1"])
            P.act(g1[:], g1[:], AF.Tanh, ["g1"], ["g1"], scale=1.0 / 15.0)
            P.act(g2[:], g1[:], AF.Exp, ["g1"], ["g2"], scale=-15.0)
            P.act(g3[:], g2[:], AF.Ln, ["g2"], ["g3"], bias=1.0)
            gov = go[:].rearrange("p (d k h) -> p d k h", d=2, k=2)
            P.ts(gov[:, :, 0, :], g1[:].rearrange("p (d k h) -> p d k h", d=2, k=2)[:, :, 0, :], 15.0, None, ALU.mult, None,
                 ["g1"], ["go"])
            P.ts(gov[:, :, 1, :], g3[:].rearrange("p (d k h) -> p d k h", d=2, k=2)[:, :, 1, :], -1.0, None, ALU.mult, None,
                 ["g3"], ["go"])
            r0 = g0 + blk * 128
            P.dma(sc["gat"][r0:r0 + 128, :], go[:], ["go"], [])
    P.S.barrier()
    A.release(mk)


def scan_alloc(P):
    A = P.A
    st = {}
    st["C"] = A.alloc([128, 8, 256], F32, "C")
    st["Cb"] = A.alloc([128, 8, 256], BF16, "Cb")
    st["n"] = A.alloc([128, 8], F32, "n")
    st["nb"] = A.alloc([128, 8], BF16, "nb")
    st["m"] = A.alloc([128, 8], F32, "m")
    st["in"] = [dict(q=A.alloc([128, 8, 64], BF16, "sq"), k=A.alloc([128, 8, 64], BF16, "sk"),
                     ktm=A.alloc([64, 8, 128], BF16, "sktm"), vtm=A.alloc([64, 8, 256], BF16, "svtm"),
                     g=A.alloc([64, 32], F32, "sg")) for _ in range(2)]
    for nm_, shp, dt in [("bcol", [64, 8], F32), ("a", [64, 8], F32), ("bend", [128, 8], F32), ("Da", [64, 8, 64], F32),
                         ("tmp", [64, 8, 64], F32), ("cm", [64, 8], F32), ("u", [64, 8], F32), ("Du", [64, 8, 64], F32),
                         ("e1", [64, 8, 64], F32), ("dT", [64, 8, 64], F32), ("e2", [128, 8, 64], F32),
                         ("qs", [128, 8, 64], BF16), ("uend", [128, 8], F32), ("mnew", [128, 8], F32),
                         ("dec", [128, 8], F32), ("w", [64, 8], F32), ("enm", [64, 8], F32), ("smT", [64, 8, 64], BF16),
                         ("dn", [64, 8], F32), ("kw", [64, 8, 128], BF16)]:
        st[nm_] = A.alloc(shp, dt, "s_" + nm_)
    st["hout"] = [A.alloc([64, 8, 256], F32, f"hout{i}") for i in range(2)]
    return st


def scan_init(P, st, state_in):
    if state_in is None:
        P.S.add("dve", lambda e: e.memset(st["C"][:], 0.0), writes=["C"])
        P.S.add("dve", lambda e: e.memset(st["Cb"][:], 0.0), writes=["Cb"])
        P.S.add("dve", lambda e: e.memset(st["n"][:], 0.0), writes=["n"])
        P.S.add("dve", lambda e: e.memset(st["nb"][:], 0.0), writes=["nb"])
        P.S.add("dve", lambda e: e.memset(st["m"][:], 0.0), writes=["m"])
    else:
        P.dma(st["C"][:].rearrange("p h v -> p (h v)"), state_in[:, 0:2048], [], ["C"])
        P.dma(st["n"][:], state_in[:, 2048:2056], [], ["n"])
        P.dma(st["m"][:], state_in[:, 2056:2064], [], ["m"])
        P.cp(st["Cb"][:], st["C"][:], ["C"], ["Cb"], eng="pool")
        P.cp(st["nb"][:], st["n"][:], ["n"], ["nb"])


def scan_save(P, st, state_out):
    P.dma(state_out[:, 0:2048], st["C"][:].rearrange("p h v -> p (h v)"), ["C"], ["state_out"])
    P.dma(state_out[:, 2048:2056], st["n"][:], ["n"], ["state_out"])
    P.dma(state_out[:, 2056:2064], st["m"][:], ["m"], ["state_out"])


def scan_run(P, st, sc, chunks, d, hdst):
    ones64 = P.ones_f[0:64, :]
    onesb = P.ones_b
    tri = P.tri[d]
    nm_st = P.nm[d]
    nm_ts = P.nm[1 - d]
    tend = 63 if d == 0 else 0
    pA, pAk = P.banks[6], ("ps", 6)
    P.rot = [0, 1, 2, 3, 4, 5]
    for it, j in enumerate(chunks):
        I = st["in"][it % 2]
        ik = ("sin", it % 2)
        P.dma(I["q"][:], sc["qT"][j], [], [(ik, "q")])
        P.dma(I["k"][:], sc["kT"][j], [], [(ik, "k")])
        P.dma(I["ktm"][:].rearrange("p h d -> p (h d)"), sc["ktm"][j * 64:(j + 1) * 64, :], [], [(ik, "ktm")])
        P.dma(I["vtm"][:].rearrange("p h d -> p (h d)"), sc["vtm"][j * 64:(j + 1) * 64, :], [], [(ik, "vtm")])
        P.dma(I["g"][:], sc["gat"][j * 64:(j + 1) * 64, :], [], [(ik, "g")])
        gi = I["g"][:, d * 16:d * 16 + 8]
        gf = I["g"][:, d * 16 + 8:d * 16 + 16]
        P.mm(pA[0:64, 0:8], tri, gf, True, True, [(ik, "g")], [pAk])
        P.mm(pA[:, 8:16], ones64, gf, True, True, [(ik, "g")], [pAk])
        P.cp(st["bcol"][:], pA[0:64, 0:8], [pAk], ["bcol"])
        P.tt(st["a"][:], gi, pA[0:64, 0:8], ALU.subtract, [pAk, (ik, "g")], ["a"])
        P.cp(st["bend"][:], pA[:, 8:16], [pAk], ["bend"])
        P.tt(st["Da"][:], bcast(P.id64, 1, [64, 8, 64]), bcast(st["a"][:], 2, [64, 8, 64]), ALU.mult, ["a"], ["Da"])
        pB, pBk = P.ps()
        P.mm(pB[:, :], ones64, st["Da"][:].rearrange("p h t -> p (h t)"), True, True, ["Da"], [pBk])
        P.tt(st["tmp"][:], pB[0:64, :].rearrange("p (h t) -> p h t", h=8), bcast(nm_ts, 1, [64, 8, 64]), ALU.add, [pBk], ["tmp"])
        P.red(st["cm"][:], st["tmp"][:], ALU.max, ["tmp"], ["cm"])
        P.tt(st["u"][:], st["cm"][:], st["m"][0:64, :], ALU.max, ["cm", "m"], ["u"])
        P.tt(st["Du"][:], bcast(P.id64, 1, [64, 8, 64]), bcast(st["u"][:], 2, [64, 8, 64]), ALU.mult, ["u"], ["Du"])
        pC, pCk = P.ps()
        P.mm(pC[:, :], ones64, st["Du"][:].rearrange("p h t -> p (h t)"), True, True, ["Du"], [pCk])
        pCv = pC[:, :].rearrange("p (h t) -> p h t", h=8)
        P.stt(st["e1"][:], pCv[0:64], -1.0, bcast(nm_st, 1, [64, 8, 64]), ALU.mult, ALU.add, [pCk], ["e1"])
        P.tt(st["e1"][:], st["e1"][:], bcast(st["a"][:], 2, [64, 8, 64]), ALU.add, ["e1", "a"], ["e1"])
        P.act(st["dT"][:], st["e1"][:], AF.Exp, ["e1"], ["dT"])
        P.stt(st["e2"][:], pCv, -1.0, bcast(st["m"][:], 2, [128, 8, 64]), ALU.mult, ALU.add, [pCk, "m"], ["e2"])
        P.act(st["e2"][:], st["e2"][:], AF.Exp, ["e2"], ["e2"])
        P.tt(st["qs"][:], I["q"][:], st["e2"][:], ALU.mult, ["e2", (ik, "q")], ["qs"])
        P.cp(st["uend"][:], pCv[:, :, tend], [pCk], ["uend"])
        P.tt(st["mnew"][:], st["bend"][:], st["uend"][:], ALU.add, ["bend", "uend"], ["mnew"])
        P.tt(st["dec"][:], st["m"][:], st["uend"][:], ALU.subtract, ["m", "uend"], ["dec"])
        P.act(st["dec"][:], st["dec"][:], AF.Exp, ["dec"], ["dec"])
        P.tt(st["w"][:], st["a"][:], st["uend"][0:64, :], ALU.subtract, ["a", "uend"], ["w"])
        P.act(st["w"][:], st["w"][:], AF.Exp, ["w"], ["w"])
        P.tt(st["enm"][:], st["bcol"][:], st["u"][:], ALU.add, ["bcol", "u"], ["enm"])
        P.act(st["enm"][:], st["enm"][:], AF.Exp, ["enm"], ["enm"], scale=-1.0)
        pS, pSk = P.ps()
        for h in range(8):
            P.mm(pS[0:64, h * 64:(h + 1) * 64], I["k"][:, h, :], I["q"][:, h, :], True, True, [(ik, "k"), (ik, "q")], [pSk])
        P.tt(st["smT"][:], pS[0:64, :].rearrange("p (h t) -> p h t", h=8), st["dT"][:], ALU.mult, [pSk, "dT"], ["smT"])
        for h in range(8):
            P.mm(pA[0:64, 16 + h:17 + h], st["smT"][:, h, :], onesb[0:64, 0:1], True, False, ["smT"], [pAk])
            P.mm(pA[0:64, 16 + h:17 + h], st["qs"][:, h, :], st["nb"][:, h:h + 1], False, True, ["qs", "nb"], [pAk])
        P.act(st["dn"][:], pA[0:64, 16:24], AF.Abs, [pAk], ["dn"])
        P.tt(st["dn"][:], st["dn"][:], st["enm"][:], ALU.max, ["dn", "enm"], ["dn"])
        P.S.add("dve", lambda e: e.reciprocal(out=st["dn"][:], in_=st["dn"][:]), reads=["dn"], writes=["dn"])
        ho = st["hout"][it % 2]
        hk_ = ("hout", it % 2)
        for hp in range(4):
            pN, pNk = P.ps()
            for hh in range(2):
                h = hp * 2 + hh
                P.mm(pN[0:64, hh * 256:(hh + 1) * 256], st["smT"][:, h, :], I["vtm"][:, h, :], True, False, ["smT", (ik, "vtm")], [pNk])
                P.mm(pN[0:64, hh * 256:(hh + 1) * 256], st["qs"][:, h, :], st["Cb"][:, h, :], False, True, ["qs", "Cb"], [pNk])
            P.tt(ho[:, hp * 2:hp * 2 + 2, :], pN[0:64, :].rearrange("p (h v) -> p h v", h=2),
                 bcast(st["dn"][:, hp * 2:hp * 2 + 2], 2, [64, 2, 256]), ALU.mult, [pNk, "dn"], [hk_])
        P.dma(hdst[j * 64:(j + 1) * 64, :], ho[:].rearrange("p h v -> p (h v)"), [hk_], [])
        P.tt(st["kw"][:], I["ktm"][:], bcast(st["w"][:], 2, [64, 8, 128]), ALU.mult, [(ik, "ktm"), "w"], ["kw"], eng="pool")
        for h in range(8):
            P.mm(pA[:, 24 + h:25 + h], st["kw"][:, h, :], onesb[0:64, 0:1], True, True, ["kw"], [pAk])
        for hp in range(4):
            pU, pUk = P.ps()
            for hh in range(2):
                h = hp * 2 + hh
                P.mm(pU[:, hh * 256:(hh + 1) * 256], st["kw"][:, h, :], I["vtm"][:, h, :], True, True, ["kw", (ik, "vtm")], [pUk])
            for hh in range(2):
                h = hp * 2 + hh
                P.stt(st["C"][:, h, :], st["C"][:, h, :], st["dec"][:, h:h + 1], pU[:, hh * 256:(hh + 1) * 256], ALU.mult, ALU.add,
                      ["C", "dec", pUk], ["C"])
        P.act(st["Cb"][:], st["C"][:], AF.Copy, ["C"], ["Cb"])
        P.tt(st["n"][:], st["n"][:], st["dec"][:], ALU.mult, ["n", "dec"], ["n"])
        P.tt(st["n"][:], st["n"][:], pA[:, 24:32], ALU.add, ["n", pAk], ["n"])
        P.cp(st["nb"][:], st["n"][:], ["n"], ["nb"])
        P.cp(st["m"][:], st["mnew"][:], ["mnew"], ["m"])
    P.rot = list(range(7))


def pass2(P, xT, ctxT, sc, w_mout, w_fin, w_fout, w_qkv, ghead, gqk, cosT, sinT):
    A = P.A
    mk = A.mark()
    X = A.alloc([128, KC, 512], F32, "X")
    Y = A.alloc([128, KC, 512], F32, "Y")
    HB = A.alloc([128, KC, 512], BF16, "HB")
    P.sgb = [A.alloc([128, 512], BF16, f"sgb{i}") for i in range(2)]
    gq = A.alloc([128, 2], F32, "gq")
    P.dma(gq[:], gqk, [], ["gq"])
    mk2 = A.mark()
    for ti in range(5):
        T, g0 = tile_info(ti)
        nblk = T // 128
        v = 1 if ti == 0 else 0
        load_x_tile(P, X, "X", xT, ctxT, ti)
        A.release(mk2)
        gh = A.alloc([128, 2048], F32, "gh")
        P.dma(gh[:], ghead.partition_broadcast(128).rearrange("p a n -> p (a n)"), [], ["gh"])
        hb = [dict(h1=A.alloc([128, 8, 256], F32, "h1"), h2=A.alloc([128, 8, 256], F32, "h2"),
                   so=A.alloc([128, 2048], BF16, "so")) for _ in range(2)]
        sqh = A.alloc([128, 8, 256], F32, "sqh")
        ssq = A.alloc([128, 8], F32, "ssq")
        hg = A.alloc([128, 2048], BF16, "hg")
        for blk in range(nblk):
            B_ = hb[blk % 2]
            bk_ = ("hb", blk % 2)
            r0 = g0 + blk * 128
            P.dma(B_["h1"][:].rearrange("p h v -> p (h v)"), sc["h1"][r0:r0 + 128, :], [], [(bk_, 1)])
            P.dma(B_["h2"][:].rearrange("p h v -> p (h v)"), sc["h2"][r0:r0 + 128, :], [], [(bk_, 2)])
            P.dma(B_["so"][:], sc["sgo"][r0:r0 + 128, :], [], [(bk_, 3)])
            P.tt(B_["h1"][:], B_["h1"][:], B_["h2"][:], ALU.add, [(bk_, 1), (bk_, 2)], [(bk_, 1)], eng="pool")
            P.tt(sqh[:], B_["h1"][:], B_["h1"][:], ALU.mult, [(bk_, 1)], ["sqh"])
            P.red(ssq[:], sqh[:], ALU.add, ["sqh"], ["ssq"])
            P.ts(ssq[:], ssq[:], 1.0 / 256, EPS, ALU.mult, ALU.add, ["ssq"], ["ssq"])
            P.rsq(ssq[:], ["ssq"])
            P.tt(sqh[:], B_["h1"][:], bcast(ssq[:], 2, [128, 8, 256]), ALU.mult, [(bk_, 1), "ssq"], ["sqh"])
            P.tt(sqh[:], sqh[:], gh[:].rearrange("p (h v) -> p h v", h=8), ALU.mult, ["sqh", "gh"], ["sqh"], eng="pool")
            P.tt(hg[:], sqh[:].rearrange("p h v -> p (h v)"), B_["so"][:], ALU.mult, ["sqh", (bk_, 3)], ["hg"])
            for c4 in range(4):
                bank, bkk = P.ps()
                pb = bank.bitcast(BF16)
                for cc in range(4):
                    c = c4 * 4 + cc
                    P.tr(pb[:, cc * 128:(cc + 1) * 128], hg[:, c * 128:(c + 1) * 128], P.identb[:], ["hg"], [bkk])
                for cc in range(4):
                    c = c4 * 4 + cc
                    P.act(HB[:, c, blk * 128:(blk + 1) * 128], pb[:, cc * 128:(cc + 1) * 128], AF.Copy, [bkk], [("HB", c)])
        P.linear_post_res(HB, "HB", KC, w_mout, X, "X", Y, "Y", T, P.modv(0, v, 2))
        P.S.barrier()
        A.release(mk2)
        ACT_T = A.alloc([128, FC, 512], BF16, "ACT")
        P.adaln(X, "X", Y, "Y", HB, "HB", T, P.modv(0, v, 3), P.modv(0, v, 4))
        P.ffn(HB, "HB", ACT_T, "ACT", w_fin, w_fout, X, "X", Y, "Y", T, P.modv(0, v, 5))
        P.S.barrier()
        A.release(mk2)
        if ti > 0:
            P.dma(sc["x1"].rearrange("(c p) t -> p c t", p=128)[:, :, (ti - 1) * 512:ti * 512], X[:, :, 0:T], ["X"], [])
        P.adaln(X, "X", Y, "Y", HB, "HB", T, P.modv(1, v, 0), P.modv(1, v, 1))
        Qt = A.alloc([128, 16, 512], BF16, "Qt")
        Kt = A.alloc([128, 4, 512], BF16, "Kt")
        cs = A.alloc([128, 512], F32, "cs")
        sn = A.alloc([128, 512], F32, "sn")
        yq = [A.alloc([128, 512], F32, f"yq{i}") for i in range(2)]
        qn = [A.alloc([128, 512], F32, f"qn{i}") for i in range(2)]
        qb = [A.alloc([128, 512], BF16, f"qb{i}") for i in range(2)]
        rq = [A.alloc([128, 512], F32, f"rq{i}") for i in range(2)]
        t2 = [A.alloc([128, 512], F32, f"t2{i}") for i in range(2)]
        vst = [A.alloc([128, 512], BF16, f"vst{i}") for i in range(2)]
        if ti > 0:
            P.dma(cs[:], cosT[:, (ti - 1) * 512:ti * 512], [], ["cs"])
            P.dma(sn[:], sinT[:, (ti - 1) * 512:ti * 512], [], ["sn"])
        hi = 0
        for t in range(6):
            if ti == 0 and t < 4:
                continue
            wv, wk = P.wload(w_qkv, KC, t * 512, 512)
            if t < 5:
                for jj in range(4):
                    isq = t < 4
                    hh = t * 4 + jj if isq else jj
                    i2 = hi % 2
                    hi += 1
                    bank, bk = P.ps()
                    for kc in range(KC):
                        P.mm(bank[:, 0:T], wv[:, kc, jj * 128:(jj + 1) * 128], HB[:, kc, 0:T], kc == 0, kc == KC - 1,
                             [wk, ("HB", kc)], [bk])
                    P.cp(yq[i2][:, 0:T], bank[:, 0:T], [bk], [("yq", i2)])
                    sq = P.sqc[i2]
                    P.act(sq[:, 0:T], bank[:, 0:T], AF.Square, [bk], [("sqc", i2)])
                    b2, b2k = P.ps()
                    P.mm(b2[:, 0:T], P.ones_b, sq[:, 0:T], True, True, [("sqc", i2)], [b2k])
                    P.ts(rq[i2][:, 0:T], b2[:, 0:T], 1.0 / 128, EPS, ALU.mult, ALU.add, [b2k], [("rq", i2)])
                    P.rsq(rq[i2][:, 0:T], [("rq", i2)])
                    gcol = gq[:, 0:1] if isq else gq[:, 1:2]
                    dst = Qt[:, hh, 0:T] if isq else Kt[:, hh, 0:T]
                    dk_ = ("Qt", hh) if isq else ("Kt", hh)
                    if ti == 0:
                        P.stt(dst, yq[i2][:, 0:T], gcol, rq[i2][:, 0:T], ALU.mult, ALU.mult, [("yq", i2), ("rq", i2), "gq"], [dk_])
                    else:
                        P.stt(qn[i2][:, 0:T], yq[i2][:, 0:T], gcol, rq[i2][:, 0:T], ALU.mult, ALU.mult,
                              [("yq", i2), ("rq", i2), "gq"], [("qn", i2)])
                        P.act(qb[i2][:, 0:T], qn[i2][:, 0:T], AF.Copy, [("qn", i2)], [("qb", i2)])
                        b3, b3k = P.ps()
                        P.mm(b3[:, 0:T], P.pswap_b, qb[i2][:, 0:T], True, True, [("qb", i2)], [b3k])
                        P.tt(t2[i2][:, 0:T], b3[:, 0:T], sn[:, 0:T], ALU.mult, [b3k, "sn"], [("t2", i2)])
                        P.tt(qn[i2][:, 0:T], qn[i2][:, 0:T], cs[:, 0:T], ALU.mult, [("qn", i2), "cs"], [("qn", i2)], eng="pool")
                        P.tt(dst, qn[i2][:, 0:T], t2[i2][:, 0:T], ALU.add, [("qn", i2), ("t2", i2)], [dk_])
            else:
                for blk in range(nblk):
                    bank, bk = P.ps()
                    for kc in range(KC):
                        P.mm(bank[:, :], HB[:, kc, blk * 128:(blk + 1) * 128], wv[:, kc, :], kc == 0, kc == KC - 1,
                             [wk, ("HB", kc)], [bk])
                    i2 = blk % 2
                    P.act(vst[i2][:], bank[:, :], AF.Copy, [bk], [("vst", i2)])
                    r0 = g0 + blk * 128
                    P.dma(sc["V"][r0:r0 + 128, :], vst[i2][:], [("vst", i2)], [])
        if ti > 0:
            P.dma(sc["Q"][ti - 1], Qt[:], [("Qt", h) for h in range(16)], [])
        P.dma(sc["KT"][:, :, g0:g0 + T], Kt[:, :, 0:T], [("Kt", h) for h in range(4)], [])
        P.S.barrier()
    A.release(mk)


def pass3(P, sc, kt_oth, v_oth, w_aout, w_fin, w_fout, outT):
    A = P.A
    NK = NTOK + NLAT
    NKC = NK // 128
    HB = A.alloc([128, KC, 512], BF16, "HB")
    mk2 = A.mark()
    T = 512
    scale = 128.0 ** -0.5
    for ti in range(1, 5):
        A.release(mk2)
        KTa = A.alloc([128, 4, NK], BF16, "KTa")
        Va = A.alloc([128, NKC, 512], BF16, "Va")
        Qt = A.alloc([128, 16, 512], BF16, "Qt")
        pT = [A.alloc([128, 512], BF16, f"pT{i}") for i in range(4)]
        rsum = A.alloc([128, 512], F32, "rsum")
        for kv in range(4):
            P.dma(KTa[:, kv, 0:NTOK], sc["KT"][:, kv, :], [], ["KTa"])
            P.dma(KTa[:, kv, NTOK:NK], kt_oth[:, kv, NCTX:NTOK], [], ["KTa"])
        P.dma(Va[:, 0:NTOK // 128, :], sc["V"].rearrange("(c p) n -> p c n", p=128), [], ["Va"])
        P.dma(Va[:, NTOK // 128:NKC, :], v_oth[NCTX:NTOK, :].rearrange("(c p) n -> p c n", p=128), [], ["Va"])
        P.dma(Qt[:], sc["Q"][ti - 1], [], ["Qt"])
        P.rot = [0, 1, 2, 3]
        for h in range(16):
            kv = h // 4
            po, pok = P.banks[4 + (h % 2)], ("ps", 4 + (h % 2))
            psm, psk = P.banks[6 + (h % 2)], ("ps", 6 + (h % 2))
            sb = [None] * NKC

            def s_mm(kc):
                bank, bk = P.ps()
                P.mm(bank[:, :], KTa[:, kv, kc * 128:(kc + 1) * 128], Qt[:, h, :], True, True, ["KTa", "Qt"], [bk])
                sb[kc] = (bank, bk)

            s_mm(0)
            s_mm(1)
            for kc in range(NKC):
                bank, bk = sb[kc]
                p_ = pT[kc % 4]
                pk = ("pT", kc % 4)
                P.act(p_[:], bank[:, :], AF.Exp, [bk], [pk], scale=scale)
                if kc + 2 < NKC:
                    s_mm(kc + 2)
                P.mm(po[:, :], Va[:, kc, kv * 128:(kv + 1) * 128], p_[:], kc == 0, kc == NKC - 1, ["Va", pk], [pok])
                P.mm(psm[:, :], P.ones_b, p_[:], kc == 0, kc == NKC - 1, [pk], [psk])
            P.S.add("dve", lambda e, psm=psm: e.reciprocal(out=rsum[:], in_=psm[:, :]), reads=[psk], writes=["rsum"])
            P.tt(HB[:, h, :], po[:, :], rsum[:], ALU.mult, [pok, "rsum"], [("HB", h)])
        P.rot = list(range(7))
        P.S.barrier()
        A.release(mk2)
        X = A.alloc([128, KC, 512], F32, "X")
        Y = A.alloc([128, KC, 512], F32, "Y")
        P.sgb = [A.alloc([128, 512], BF16, f"sgb{i}") for i in range(2)]
        ACT_T = A.alloc([128, FC, 512], BF16, "ACT")
        P.dma(X[:], sc["x1"].rearrange("(c p) t -> p c t", p=128)[:, :, (ti - 1) * 512:ti * 512], [], ["X"])
        P.linear_post_res(HB, "HB", KC, w_aout, X, "X", Y, "Y", T, P.modv(1, 0, 2))
        P.adaln(X, "X", Y, "Y", HB, "HB", T, P.modv(1, 0, 3), P.modv(1, 0, 4))
        P.ffn(HB, "HB", ACT_T, "ACT", w_fin, w_fout, X, "X", Y, "Y", T, P.modv(1, 0, 5))
        P.dma(outT.rearrange("(c p) t -> p c t", p=128)[:, :, (ti - 1) * 512:ti * 512], X[:], ["X"], ["out"])
        P.S.barrier()


def build_stage(stage):
    P = Prog(stage)
    P.setup_consts()
    alloc_common(P)
    sc = {}
    S1, S2, S3 = 1, 2, 3
    if stage in (1, 2, 0):
        xT = P.din("xT", [D, NLAT])
        ctxT = P.din("ctxT", [D, NCTX])
    if stage in (1, 0):
        cond = P.din("cond", [128, 32])
        w_mod = [P.din(f"w_mod{l}", [D, 6 * D]) for l in range(2)]
        b_mod = P.din("b_mod", [128, 192])
        gvec = P.din("gvec", [128, 128])
        w_in = P.din("w_mlstm_in", [D, 6176])
        bgate = P.din("bgate", [1, 32])
    s_mod = P.scr("s_mod", [128, 384], F32, S1, [S2, S3])
    sc["qT"] = P.scr("s_qT", [NCHUNK, 128, 8, 64], BF16, S1, [S1, S2])
    sc["kT"] = P.scr("s_kT", [NCHUNK, 128, 8, 64], BF16, S1, [S1, S2])
    sc["ktm"] = P.scr("s_ktm", [NTOK, 1024], BF16, S1, [S1, S2])
    sc["vtm"] = P.scr("s_vtm", [NTOK, 2048], BF16, S1, [S1, S2])
    sc["sgo"] = P.scr("s_sgo", [NTOK, 2048], BF16, S1, [S2])
    sc["gat"] = P.scr("s_gat", [NTOK, 32], F32, S1, [S1, S2])
    sc["h1"] = P.scr("s_h1", [NTOK, 2048], F32, S1, [S2])
    sc["h2"] = P.scr("s_h2", [NTOK, 2048], F32, S2, [S2])
    state_out = P.scr("s_state", [128, 2064], F32, S1, [S1, S2 + 10])
    sc["x1"] = P.scr("s_x1", [D, NLAT], F32, S2, [S3])
    sc["Q"] = P.scr("s_Q", [4, 128, 16, 512], BF16, S2, [S3])
    sc["KT"] = P.scr("s_KT", [128, 4, NTOK], BF16, S2, [S3])
    sc["V"] = P.scr("s_V", [NTOK, 512], BF16, S2, [S3])

    if stage == 1:
        P.phase_mod(w_mod, b_mod, gvec, cond, s_mod)
        pass1(P, xT, ctxT, w_in, bgate, sc)
        mk = P.A.mark()
        st = scan_alloc(P)
        scan_init(P, st, None)
        scan_run(P, st, sc, list(range(0, NCHUNK)), 0, sc["h1"])
        scan_save(P, st, state_out)
        P.S.barrier()
        P.A.release(mk)
    elif stage == 2:
        P.dma(P.MOD[:], s_mod, [], ["MOD"])
        state_in = P.din("state_in", [128, 2064])
        w_mout = P.din("w_mlstm_out", [D, D])
        w_fin = P.din("w_ffn_in", [D, 2 * FH])
        w_fout = P.din("w_ffn_out", [FH, D])
        w_qkv = P.din("w_attn_qkv", [D, 3072])
        ghead = P.din("ghead", [1, 2048])
        gqk = P.din("gqk", [128, 2])
        cosT = P.din("cosT", [128, NLAT])
        sinT = P.din("sinT", [128, NLAT])
        mk = P.A.mark()
        st = scan_alloc(P)
        scan_init(P, st, None)
        scan_run(P, st, sc, list(range(3, -1, -1)), 1, sc["h2"])
        scan_init(P, st, state_in)
        scan_run(P, st, sc, list(range(NCHUNK - 1, 3, -1)), 1, sc["h2"])
        P.S.barrier()
        P.A.release(mk)
        pass2(P, xT, ctxT, sc, w_mout, w_fin, w_fout, w_qkv, ghead, gqk, cosT, sinT)
    elif stage == 3:
        P.dma(P.MOD[:], s_mod, [], ["MOD"])
        kt_oth = P.din("KT_oth", [128, 4, NTOK], BF16)
        v_oth = P.din("V_oth", [NTOK, 512], BF16)
        w_aout = P.din("w_attn_out", [D, D])
        w_fin = P.din("w_ffn_in", [D, 2 * FH])
        w_fout = P.din("w_ffn_out", [FH, D])
        outT = P.dout("outT", [D, NLAT])
        pass3(P, sc, kt_oth, v_oth, w_aout, w_fin, w_fout, outT)
    P.S.barrier()
    P.S.emit()
    return P


def make_consts():
    c = np.zeros((128, 128 * 7), np.float32)
    c[:, 0:128] = 1.0
    sw = np.zeros((128, 128), np.float32)
    for i in range(64):
        sw[2 * i, 2 * i + 1] = 1.0
        sw[2 * i + 1, 2 * i] = 1.0
    c[:, 128:256] = sw
    s = np.arange(64)[:, None]
    t = np.arange(64)[None, :]
    c[0:64, 256:320] = (s <= t)
    c[0:64, 320:384] = (s >= t)
    c[0:64, 384:448] = np.where(s <= t, 0.0, NEG)
    c[0:64, 448:512] = np.where(s >= t, 0.0, NEG)
    c[0:64, 512:576] = np.eye(64)
    c[:, 768:896] = np.eye(128)
    return c


def rope_tables(pos):
    row = (pos // 64).astype(np.float32)
    col = (pos % 64).astype(np.float32)
    inv = (10000.0 ** (-np.arange(0, 64, 2, dtype=np.float32) / 64)).astype(np.float32)
    ang = np.concatenate([row[:, None] * inv, col[:, None] * inv], axis=-1).astype(np.float32)
    cos = np.cos(ang).astype(np.float32)
    sin = np.sin(ang).astype(np.float32)
    cf = np.repeat(cos, 2, axis=1).T
    sf = np.repeat(sin, 2, axis=1).T.copy()
    sf[0::2, :] *= -1.0
    return np.ascontiguousarray(cf), np.ascontiguousarray(sf)


_PROGS = {}
_STOP_AFTER = 0
_R1 = None
_R2 = None


def _prog(stage):
    if stage not in _PROGS:
        _PROGS[stage] = build_stage(stage)
    return _PROGS[stage]


def fm(vec):
    return np.ascontiguousarray(np.asarray(vec, np.float32).reshape(16, 128).T)


def kernel(x, c, ctx, c_ctx, w_mod, b_mod, g_mix_pre, g_mix_post, g_ffn_pre, g_ffn_post,
           w_mlstm_in, b_mlstm_gate, g_mlstm_head, w_mlstm_out,
           w_attn_qkv, g_attn_q, g_attn_k, w_attn_out, w_ffn_in, w_ffn_out):
    f32 = np.float32
    x = np.asarray(x, f32); ctx = np.asarray(ctx, f32); c = np.asarray(c, f32); c_ctx = np.asarray(c_ctx, f32)
    w_mod = np.asarray(w_mod, f32); b_mod = np.asarray(b_mod, f32)
    w_mlstm_in = np.asarray(w_mlstm_in, f32)[0]
    w_ffn_in = np.asarray(w_ffn_in, f32); w_ffn_out = np.asarray(w_ffn_out, f32)
    cores = list(range(8))
    cst = make_consts()
    perm = np.arange(6176)
    perm[6144:6160], perm[6160:6176] = np.arange(6160, 6176), np.arange(6144, 6160)
    w_in_B = np.ascontiguousarray(w_mlstm_in[:, perm])
    bg = np.asarray(b_mlstm_gate, f32).reshape(1, 32)
    bg_B = np.ascontiguousarray(bg[:, perm[6144:] - 6144])
    gvec = np.concatenate([fm(g[l]) for g in (g_mix_pre, g_mix_post, g_ffn_pre, g_ffn_post) for l in range(2)], axis=1)
    bmod = np.concatenate([np.ascontiguousarray(np.asarray(b_mod[l], f32).reshape(96, 128).T) for l in range(2)], axis=1)
    w_mod0 = np.ascontiguousarray(w_mod[0]); w_mod1 = np.ascontiguousarray(w_mod[1])
    xTs, ctxTs, conds, poss = [], [], [], []
    for core in cores:
        b, half = core // 2, core % 2
        if half == 0:
            xs = x[b, :NLAT]; cs_ = ctx[b]; pos = np.arange(0, NLAT)
        else:
            xs = x[b, NLAT:][::-1]; cs_ = ctx[b][::-1]; pos = np.arange(NLAT, 2 * NLAT)[::-1]
        xTs.append(np.ascontiguousarray(xs.T)); ctxTs.append(np.ascontiguousarray(cs_.T)); poss.append(pos)
        cd = np.stack([c[b], c_ctx], axis=1)
        conds.append(np.ascontiguousarray(cd.reshape(16, 128, 2).transpose(1, 0, 2).reshape(128, 32)))
    if _R1 is not None:
        r1 = _R1
    P1 = _prog(1) if _R1 is None else None
    in1 = []
    for core in cores:
        half = core % 2
        in1.append({"cst": cst, "xT": xTs[core], "ctxT": ctxTs[core], "cond": conds[core], "w_mod0": w_mod0, "w_mod1": w_mod1,
                    "b_mod": bmod, "gvec": gvec, "w_mlstm_in": w_in_B if half else w_mlstm_in, "bgate": bg_B if half else bg})
    if _R1 is None:
        r1 = run_bass_kernel_spmd(P1.nc, in1, core_ids=cores).results
    if _STOP_AFTER == 1:
        return r1
    P2 = _prog(2)
    gqk = np.ascontiguousarray(np.stack([np.asarray(g_attn_q, f32)[0], np.asarray(g_attn_k, f32)[0]], axis=1))
    ghead = np.asarray(g_mlstm_head, f32).reshape(1, 2048)
    w_mout = np.ascontiguousarray(np.asarray(w_mlstm_out, f32)[0])
    w_qkv = np.ascontiguousarray(np.asarray(w_attn_qkv, f32)[0])
    wfi0 = np.ascontiguousarray(w_ffn_in[0]); wfo0 = np.ascontiguousarray(w_ffn_out[0])
    in2 = []
    for core in cores:
        cf, sf = rope_tables(poss[core])
        d_ = {"cst": cst, "xT": xTs[core], "ctxT": ctxTs[core], "state_in": r1[core ^ 1]["s_state"],
              "w_mlstm_out": w_mout, "w_ffn_in": wfi0, "w_ffn_out": wfo0, "w_attn_qkv": w_qkv, "ghead": ghead, "gqk": gqk,
              "cosT": cf, "sinT": sf}
        for nm_ in ("s_mod", "s_qT", "s_kT", "s_ktm", "s_vtm", "s_sgo", "s_gat", "s_h1"):
            d_[nm_] = r1[core][nm_]
        in2.append(d_)
    if _R2 is not None:
        r2 = _R2
    else:
        r2 = run_bass_kernel_spmd(P2.nc, in2, core_ids=cores).results
    if _STOP_AFTER == 2:
        return r1, r2
    P3 = _prog(3)
    w_aout = np.ascontiguousarray(np.asarray(w_attn_out, f32)[0])
    wfi1 = np.ascontiguousarray(w_ffn_in[1]); wfo1 = np.ascontiguousarray(w_ffn_out[1])
    in3 = []
    for core in cores:
        d_ = {"cst": cst, "s_mod": r1[core]["s_mod"], "KT_oth": r2[core ^ 1]["s_KT"], "V_oth": r2[core ^ 1]["s_V"],
              "w_attn_out": w_aout, "w_ffn_in": wfi1, "w_ffn_out": wfo1}
        for nm_ in ("s_x1", "s_Q", "s_KT", "s_V"):
            d_[nm_] = r2[core][nm_]
        in3.append(d_)
    r3 = run_bass_kernel_spmd(P3.nc, in3, core_ids=cores).results
    out = np.empty((4, 2 * NLAT, D), f32)
    for core in cores:
        b, half = core // 2, core % 2
        y = np.asarray(r3[core]["outT"], f32).T
        if half == 0:
            out[b, :NLAT] = y
        else:
            out[b, NLAT:] = y[::-1]
    return out
```
